# Optimizing a Trainium2 kernel written in Bass

```python
import jax, jax.numpy as jnp
from jax import lax
import numpy as np

D_MODEL = 2048
BATCH = 8
SEQ = 4096
DEPTH = 2

MEM_LEN = 256
N_MIXERS = 2
CHUNK = 64
EPS = 1e-6

XA_HEADS = 4
XA_WIDTH = D_MODEL // 4
XA_HEAD_DIM = XA_WIDTH // XA_HEADS
MIX_WIDTH = D_MODEL - XA_WIDTH

A_HEADS = 4
A_V_DIM = MIX_WIDTH // A_HEADS
A_QK_DIM = A_V_DIM // 2
A_CONV = 4

B_EXPAND = 128
B_HEADS = MIX_WIDTH // B_EXPAND
B_K_DIM = B_EXPAND
B_V_DIM = MIX_WIDTH // B_HEADS

D_FF = 5632
FFN_CONV = 3

N_A = (DEPTH + 1) // 2
N_B = DEPTH // 2
COLS_A = 2 * A_HEADS * A_QK_DIM + 2 * MIX_WIDTH + 2 * A_HEADS + XA_WIDTH
COLS_B = 2 * B_HEADS * B_K_DIM + 2 * MIX_WIDTH + XA_WIDTH

kernel_name = 'hybrid_mlstm_hgrn2_memxattn_convffn'


def rmsnorm(x, g):
    xf = x.astype(jnp.float32)
    y = xf * lax.rsqrt(jnp.mean(xf * xf, axis=-1, keepdims=True) + EPS)
    return (y * g.astype(jnp.float32)).astype(x.dtype)


def head_rmsnorm(o, g):
    o = o * lax.rsqrt(jnp.mean(o * o, axis=-1, keepdims=True) + EPS)
    b, s, h, d = o.shape
    return o.reshape(b, s, h * d) * g.astype(jnp.float32)


def causal_dwconv(x, w, b):
    width = w.shape[0]
    s = x.shape[1]
    xp = jnp.pad(x, ((0, 0), (width - 1, 0), (0, 0)))
    y = b
    for k in range(width):
        y = y + w[k] * xp[:, k:k + s]
    return y


def _to_chunks(t):
    b, s = t.shape[:2]
    t = t.reshape((b, s // CHUNK, CHUNK) + t.shape[2:])
    return jnp.swapaxes(jnp.moveaxis(t, 1, 0), 2, 3)


def _from_chunks(t):
    t = jnp.moveaxis(jnp.swapaxes(t, 2, 3), 0, 1)
    b, nc, l = t.shape[:3]
    return t.reshape((b, nc * l) + t.shape[3:])


def mlstm_chunkwise(q, k, v, ig, lf):
    b, s, h, dk = q.shape
    dv = v.shape[-1]
    causal = jnp.tril(jnp.ones((CHUNK, CHUNK), dtype=bool))

    def step(carry, xs):
        c_st, n_st, m_st = carry
        qc, kc, vc, ic, fc = xs
        g = jnp.cumsum(fc, axis=-1)
        a = g + m_st[..., None]
        dmat = g[..., :, None] - g[..., None, :] + ic[..., None, :]
        dmat = jnp.where(causal, dmat, -jnp.inf)
        m_row = jnp.maximum(a, jnp.max(dmat, axis=-1))
        w_inter = jnp.exp(a - m_row)
        sc = jnp.einsum('bhld,bhsd->bhls', qc, kc) * jnp.exp(dmat - m_row[..., None])
        num = (w_inter[..., None] * jnp.einsum('bhld,bhdv->bhlv', qc, c_st)
               + jnp.einsum('bhls,bhsv->bhlv', sc, vc))
        den = w_inter * jnp.einsum('bhld,bhd->bhl', qc, n_st) + jnp.sum(sc, axis=-1)
        out = num / jnp.maximum(jnp.abs(den), jnp.exp(-m_row))[..., None]
        a_end = g[..., -1] + m_st
        w_end = g[..., -1:] - g + ic
        m_new = jnp.maximum(a_end, jnp.max(w_end, axis=-1))
        decay = jnp.exp(a_end - m_new)
        ws = jnp.exp(w_end - m_new[..., None])
        c_new = decay[..., None, None] * c_st + jnp.einsum('bhl,bhld,bhlv->bhdv', ws, kc, vc)
        n_new = decay[..., None] * n_st + jnp.einsum('bhl,bhld->bhd', ws, kc)
        return (c_new, n_new, m_new), out

    init = (jnp.zeros((b, h, dk, dv), jnp.float32), jnp.zeros((b, h, dk), jnp.float32),
            jnp.zeros((b, h), jnp.float32))
    xs = (_to_chunks(q), _to_chunks(k), _to_chunks(v), _to_chunks(ig), _to_chunks(lf))
    _, out = lax.scan(step, init, xs)
    return _from_chunks(out)


def hgrn2_chunkwise(q, k, v, lf):
    b, s, h, dk = q.shape
    dv = v.shape[-1]
    causal = jnp.tril(jnp.ones((CHUNK, CHUNK), dtype=bool))

    def step(s_st, xs):
        qc, kc, vc, fc = xs
        gcum = jnp.cumsum(fc, axis=2)
        diff = gcum[:, :, :, None, :] - gcum[:, :, None, :, :]
        decay = jnp.exp(jnp.where(causal[..., None], diff, -jnp.inf))
        att = jnp.einsum('bhld,bhlsd,bhsd->bhls', qc, decay, kc)
        out = (jnp.einsum('bhld,bhdv->bhlv', qc * jnp.exp(gcum), s_st)
               + jnp.einsum('bhls,bhsv->bhlv', att, vc))
        g_end = gcum[:, :, -1]
        kd = kc * jnp.exp(g_end[:, :, None, :] - gcum)
        s_new = jnp.exp(g_end)[..., None] * s_st + jnp.einsum('bhld,bhlv->bhdv', kd, vc)
        return s_new, out

    init = jnp.zeros((b, h, dk, dv), jnp.float32)
    xs = (_to_chunks(q), _to_chunks(k), _to_chunks(v), _to_chunks(lf))
    _, out = lax.scan(step, init, xs)
    return _from_chunks(out)


def mem_cross_attention(qx, mem_n, w_kv):
    b, s, _ = qx.shape
    m = mem_n.shape[1]
    kv = mem_n @ w_kv
    km, vm = jnp.split(kv, 2, axis=-1)
    q = qx.reshape(b, s, XA_HEADS, XA_HEAD_DIM)
    km = km.reshape(b, m, XA_HEADS, XA_HEAD_DIM)
    vm = vm.reshape(b, m, XA_HEADS, XA_HEAD_DIM)
    sc = jnp.einsum('bshd,bmhd->bhsm', q, km).astype(jnp.float32) * (XA_HEAD_DIM ** -0.5)
    p = jax.nn.softmax(sc, axis=-1).astype(vm.dtype)
    return jnp.einsum('bhsm,bmhd->bshd', p, vm).reshape(b, s, XA_WIDTH)


def mlstm_layer_mixer(hn, mem_n, w_in, gate_b, conv_w, conv_b, head_g, w_kv, w_out):
    b, s, _ = hn.shape
    qk_w = A_HEADS * A_QK_DIM
    z = hn @ w_in
    qk, v, o_pre, gates, xq = jnp.split(
        z, [2 * qk_w, 2 * qk_w + MIX_WIDTH, 2 * qk_w + 2 * MIX_WIDTH,
            2 * qk_w + 2 * MIX_WIDTH + 2 * A_HEADS], axis=-1)
    qk = jax.nn.silu(causal_dwconv(qk, conv_w, conv_b))
    q, k = jnp.split(qk, 2, axis=-1)
    gates = gates.astype(jnp.float32) + gate_b.astype(jnp.float32)
    ig = gates[..., :A_HEADS]
    lf = jax.nn.log_sigmoid(gates[..., A_HEADS:])
    q = q.reshape(b, s, A_HEADS, A_QK_DIM).astype(jnp.float32)
    k = k.reshape(b, s, A_HEADS, A_QK_DIM).astype(jnp.float32) * (A_QK_DIM ** -0.5)
    v = v.reshape(b, s, A_HEADS, A_V_DIM).astype(jnp.float32)
    hh = head_rmsnorm(mlstm_chunkwise(q, k, v, ig, lf), head_g)
    y_mix = (jax.nn.sigmoid(o_pre.astype(jnp.float32)) * hh).astype(hn.dtype)
    y_mem = mem_cross_attention(xq, mem_n, w_kv)
    return jnp.concatenate([y_mix, y_mem], axis=-1) @ w_out


def hgrn2_layer_mixer(hn, mem_n, lb, w_in, head_g, w_kv, w_out):
    b, s, _ = hn.shape
    kw = B_HEADS * B_K_DIM
    z = hn @ w_in
    q, f, i, g, xq = jnp.split(z, [kw, 2 * kw, 2 * kw + MIX_WIDTH, 2 * kw + 2 * MIX_WIDTH], axis=-1)
    f = f.astype(jnp.float32)
    lf = jnp.logaddexp(jnp.log(lb), jnp.log1p(-lb) + jax.nn.log_sigmoid(f))
    kk = (1.0 - lb) * jax.nn.sigmoid(-f)
    q = jax.nn.silu(q.astype(jnp.float32)).reshape(b, s, B_HEADS, B_K_DIM)
    kk = kk.reshape(b, s, B_HEADS, B_K_DIM)
    lf = lf.reshape(b, s, B_HEADS, B_K_DIM)
    i = i.astype(jnp.float32).reshape(b, s, B_HEADS, B_V_DIM)
    o = head_rmsnorm(hgrn2_chunkwise(q, kk, i, lf), head_g)
    y_mix = (o * jax.nn.silu(g.astype(jnp.float32))).astype(hn.dtype)
    y_mem = mem_cross_attention(xq, mem_n, w_kv)
    return jnp.concatenate([y_mix, y_mem], axis=-1) @ w_out


def conv_ffn(hn, w_up, conv_w, conv_b, w_down):
    u, g = jnp.split(hn @ w_up, 2, axis=-1)
    g = causal_dwconv(g, conv_w, conv_b)
    return (jax.nn.silu(g) * u) @ w_down


def setup_inputs(seed: int = 0) -> dict:
    key = jax.random.key(seed)
    ks = jax.random.split(key, 24)
    f32 = jnp.float32

    def nrm(k, shape, scale):
        return jax.random.normal(k, shape, f32) * scale

    def gain(k, shape):
        return 1.0 + 0.02 * jax.random.normal(k, shape, f32)

    qk_w = A_HEADS * A_QK_DIM
    return {
        'x': nrm(ks[0], (BATCH, SEQ, D_MODEL), 1.0),
        'mem': nrm(ks[1], (BATCH, MEM_LEN, D_MODEL), 1.0),
        'norm_mix_g': gain(ks[2], (DEPTH, D_MODEL)),
        'norm_mem_g': gain(ks[3], (DEPTH, D_MODEL)),
        'norm_ffn_g': gain(ks[4], (DEPTH, D_MODEL)),
        'norm_out_g': gain(ks[5], (D_MODEL,)),
        'w_mem_kv': nrm(ks[6], (DEPTH, D_MODEL, 2 * XA_WIDTH), D_MODEL ** -0.5),
        'a_w_in': nrm(ks[7], (N_A, D_MODEL, COLS_A), D_MODEL ** -0.5),
        'a_gate_b': jnp.concatenate([nrm(ks[8], (N_A, A_HEADS), 0.1),
                                     3.0 + nrm(ks[9], (N_A, A_HEADS), 0.1)], axis=-1),
        'a_conv_w': nrm(ks[10], (N_A, A_CONV, 2 * qk_w), A_CONV ** -0.5),
        'a_conv_b': nrm(ks[11], (N_A, 2 * qk_w), 0.02),
        'a_head_g': gain(ks[12], (N_A, MIX_WIDTH)),
        'a_w_out': nrm(ks[13], (N_A, D_MODEL, D_MODEL), D_MODEL ** -0.5),
        'b_w_in': nrm(ks[14], (N_B, D_MODEL, COLS_B), D_MODEL ** -0.5),
        'b_lb_logits': nrm(ks[15], (DEPTH, B_HEADS * B_K_DIM), 0.1),
        'b_head_g': gain(ks[16], (N_B, MIX_WIDTH)),
        'b_w_out': nrm(ks[17], (N_B, D_MODEL, D_MODEL), D_MODEL ** -0.5),
        'ffn_w_up': nrm(ks[18], (DEPTH, D_MODEL, 2 * D_FF), D_MODEL ** -0.5),
        'ffn_conv_w': nrm(ks[19], (DEPTH, FFN_CONV, D_FF), FFN_CONV ** -0.5),
        'ffn_conv_b': nrm(ks[20], (DEPTH, D_FF), 0.02),
        'ffn_w_down': nrm(ks[21], (DEPTH, D_FF, D_MODEL), D_FF ** -0.5),
    }


def reference(x, mem, norm_mix_g, norm_mem_g, norm_ffn_g, norm_out_g, w_mem_kv,
              a_w_in, a_gate_b, a_conv_w, a_conv_b, a_head_g, a_w_out,
              b_w_in, b_lb_logits, b_head_g, b_w_out,
              ffn_w_up, ffn_conv_w, ffn_conv_b, ffn_w_down):
    lb_all = jnp.cumsum(jax.nn.softmax(b_lb_logits.astype(jnp.float32), axis=0), axis=0)
    lb_all = lb_all - lb_all[0]
    h = x
    for layer in range(DEPTH):
        hn = rmsnorm(h, norm_mix_g[layer])
        mem_n = rmsnorm(mem, norm_mem_g[layer])
        j = layer // N_MIXERS
        if layer % N_MIXERS == 0:
            y = mlstm_layer_mixer(hn, mem_n, a_w_in[j], a_gate_b[j], a_conv_w[j], a_conv_b[j],
                                  a_head_g[j], w_mem_kv[layer], a_w_out[j])
        else:
            y = hgrn2_layer_mixer(hn, mem_n, lb_all[layer], b_w_in[j], b_head_g[j],
                                  w_mem_kv[layer], b_w_out[j])
        h = h + y
        h = h + conv_ffn(rmsnorm(h, norm_ffn_g[layer]), ffn_w_up[layer], ffn_conv_w[layer],
                         ffn_conv_b[layer], ffn_w_down[layer])
    return rmsnorm(h, norm_out_g)
```

```python
import contextlib
import math
import os
import numpy as np
import concourse.bass as bass
import concourse.mybir as mybir
from concourse.bass_utils import run_bass_kernel_spmd

F32 = mybir.dt.float32
BF16 = mybir.dt.bfloat16
AF = mybir.ActivationFunctionType
ALU = mybir.AluOpType
AX = mybir.AxisListType

D = 2048
S_LEN = 4096
MEM = 256
T = 512
NTB = 4
DFF = 5632
NJ = 44
GE = 8192
EPS = 1e-6
LN_ALPHA = -0.5 * math.log(192.0)
NSLOT = 3

def _gran_cols(W, cols):
    sub = W[:, cols]
    return np.ascontiguousarray(sub.reshape(16, 128, 512).transpose(1, 0, 2).reshape(128, GE))


def _gran_down(Wd, cb, jp):
    sub = Wd[jp * 11 * 128:(jp + 1) * 11 * 128, cb * 512:(cb + 1) * 512]
    g = np.zeros((128, GE), np.float32)
    g[:, :11 * 512] = sub.reshape(11, 128, 512).transpose(1, 0, 2).reshape(128, 11 * 512)
    return g


def _a_qk_tiles():
    tiles = []
    for h in range(4):
        tiles.append(np.arange(h * 192, h * 192 + 128))
    for h in range(4):
        tiles.append(768 + np.arange(h * 192, h * 192 + 128))
    for j in range(2):
        tiles.append(np.concatenate([np.arange((2 * j) * 192 + 128, (2 * j) * 192 + 192),
                                     np.arange((2 * j + 1) * 192 + 128, (2 * j + 1) * 192 + 192)]))
    for j in range(2):
        tiles.append(768 + np.concatenate([np.arange((2 * j) * 192 + 128, (2 * j) * 192 + 192),
                                           np.arange((2 * j + 1) * 192 + 128, (2 * j + 1) * 192 + 192)]))
    return tiles


V_GMIX = 0
V_GMEM = 32
V_GFFN = 64
V_ACW = 96
V_ACB = 144
V_AHG = 156
V_BHG = 168
V_LB0 = 180
V_LB1 = 192
V_FCW = 204
V_FCB = 468
V_GATEB = 556
V_WGATE = 558
NV = 686

C_IDENT = 0
C_CAUS = 128
C_NEGM = 256
C_RST = 384
C_SEL = 896
C_ONES = 1408
NCST = 1920


def _host_prep(inp):
    f = lambda k: np.asarray(inp[k], dtype=np.float32)
    a_w_in = f("a_w_in")[0]; b_w_in = f("b_w_in")[0]
    grans = []
    wkv = f("w_mem_kv")
    for l in range(2):
        grans.append(_gran_cols(wkv[l], np.arange(0, 512)))
        grans.append(_gran_cols(wkv[l], np.arange(512, 1024)))
    qk = _a_qk_tiles()
    grans.append(_gran_cols(a_w_in, np.concatenate(qk[0:4])))
    grans.append(_gran_cols(a_w_in, np.concatenate(qk[4:8])))
    grans.append(_gran_cols(a_w_in, np.concatenate(qk[8:12])))
    grans.append(_gran_cols(a_w_in, np.arange(4616, 5128)))
    for i in range(3):
        grans.append(_gran_cols(a_w_in, 1536 + np.arange(i * 512, (i + 1) * 512)))
    for i in range(3):
        grans.append(_gran_cols(a_w_in, 3072 + np.arange(i * 512, (i + 1) * 512)))

    def common(l, w_out):
        for cb in range(4):
            grans.append(_gran_cols(w_out, np.arange(cb * 512, (cb + 1) * 512)))
        wup = f("ffn_w_up")[l]
        for gi in range(22):
            cols = np.concatenate([np.arange((2 * gi) * 128, (2 * gi + 1) * 128),
                                   DFF + np.arange((2 * gi) * 128, (2 * gi + 1) * 128),
                                   np.arange((2 * gi + 1) * 128, (2 * gi + 2) * 128),
                                   DFF + np.arange((2 * gi + 1) * 128, (2 * gi + 2) * 128)])
            grans.append(_gran_cols(wup, cols))
        wd = f("ffn_w_down")[l]
        for cb in range(4):
            for jp in range(4):
                grans.append(_gran_down(wd, cb, jp))

    common(0, f("a_w_out")[0])
    for hg in range(3):
        grans.append(_gran_cols(b_w_in, np.arange(hg * 512, (hg + 1) * 512)))
        grans.append(_gran_cols(b_w_in, 1536 + np.arange(hg * 512, (hg + 1) * 512)))
    for i in range(3):
        grans.append(_gran_cols(b_w_in, 3072 + np.arange(i * 512, (i + 1) * 512)))
    for i in range(3):
        grans.append(_gran_cols(b_w_in, 4608 + np.arange(i * 512, (i + 1) * 512)))
    grans.append(_gran_cols(b_w_in, np.arange(6144, 6656)))
    common(1, f("b_w_out")[0])
    wq32 = np.stack(grans, axis=0)

    vecs = np.zeros((128, NV), np.float32)
    pc = lambda v: v.reshape(-1, 128).T
    for l in range(2):
        vecs[:, V_GMIX + 16 * l:V_GMIX + 16 * l + 16] = pc(f("norm_mix_g")[l])
        vecs[:, V_GMEM + 16 * l:V_GMEM + 16 * l + 16] = pc(f("norm_mem_g")[l])
        vecs[:, V_GFFN + 16 * l:V_GFFN + 16 * l + 16] = pc(f("norm_ffn_g")[l])
    acw = f("a_conv_w")[0]; acb = f("a_conv_b")[0]
    for ti, cols in enumerate(qk):
        for tap in range(4):
            vecs[:, V_ACW + ti * 4 + tap] = acw[tap, cols]
        vecs[:, V_ACB + ti] = acb[cols]
    vecs[:, V_AHG:V_AHG + 12] = pc(f("a_head_g")[0])
    vecs[:, V_BHG:V_BHG + 12] = pc(f("b_head_g")[0])
    lbl = f("b_lb_logits")
    vecs[:, V_LB0:V_LB0 + 12] = pc(lbl[0]); vecs[:, V_LB1:V_LB1 + 12] = pc(lbl[1])
    fcw = f("ffn_conv_w"); fcb = f("ffn_conv_b")
    for l in range(2):
        for tap in range(3):
            o = V_FCW + (l * 3 + tap) * 44
            vecs[:, o:o + 44] = pc(fcw[l, tap])
        vecs[:, V_FCB + l * 44:V_FCB + l * 44 + 44] = pc(fcb[l])
    gb = f("a_gate_b")[0]
    vecs[0:4, V_GATEB] = gb[0:4]; vecs[0:4, V_GATEB + 1] = gb[4:8]
    wg = a_w_in[:, 4608:4616]
    vecs[:, V_WGATE:V_WGATE + 128] = wg.reshape(16, 128, 8).transpose(1, 0, 2).reshape(128, 128)

    cst = np.zeros((128, NCST), np.float32)
    cst[:, C_IDENT:C_IDENT + 128] = np.eye(128, dtype=np.float32)
    s_idx = np.arange(128)[:, None]; l_idx = np.arange(128)[None, :]
    cst[:, C_CAUS:C_CAUS + 128] = (s_idx <= l_idx).astype(np.float32)
    cst[:, C_NEGM:C_NEGM + 128] = np.where(s_idx <= l_idx, 0.0, -30000.0)
    r = np.ones(512, np.float32); r[::128] = 0.0
    cst[:, C_RST:C_RST + 512] = r[None, :]
    for h in range(4):
        cst[h, C_SEL + h * 128:C_SEL + (h + 1) * 128] = 1.0
    cst[:, C_ONES:C_ONES + 512] = 1.0
    return wq32, vecs, cst


class Res:
    __slots__ = ("name", "w", "r")

    def __init__(self, name):
        self.name = name; self.w = None; self.r = {}


class Trk:
    def __init__(self, nc, es):
        self.nc = nc
        self.eng = {"pe": nc.tensor, "act": nc.scalar, "dve": nc.vector, "pool": nc.gpsimd, "sp": nc.sync}
        self.sem = {k: es.enter_context(nc.semaphore("c_" + k)) for k in self.eng}
        self.cnt = {k: 0 for k in self.eng}
        self.waited = {k: {} for k in self.eng}
        self.dcnt = {}
        self.nw = 0

    def _wait(self, e, dep):
        sem, val = dep
        if e == "pe" and sem is self.sem["pe"]:
            return
        key = id(sem)
        if self.waited[e].get(key, 0) >= val:
            return
        self.waited[e][key] = val
        self.eng[e].wait_ge(sem, val)
        self.nw += 1

    def deps(self, e, reads, writes):
        for r in reads:
            if r.w is not None:
                self._wait(e, r.w)
        for r in writes:
            if r.w is not None:
                self._wait(e, r.w)
            for k, d in r.r.items():
                if d[0] is self.sem.get(e):
                    continue
                self._wait(e, d)

    def barrier(self):
        es_ = ["pe", "act", "dve", "pool"]
        for e in es_:
            for e2 in es_:
                if e2 != e and self.cnt[e2] > 0:
                    self._wait(e, (self.sem[e2], self.cnt[e2]))

    def _mark(self, tok, reads, writes):
        for r in writes:
            r.w = tok; r.r = {}
        for r in reads:
            k = id(tok[0])
            if k not in r.r or r.r[k][1] < tok[1]:
                r.r[k] = tok

    def op(self, e, fn, reads=(), writes=()):
        self.deps(e, reads, writes)
        ins = fn(self.eng[e])
        self.cnt[e] += 1
        ins.then_inc(self.sem[e], 1)
        tok = (self.sem[e], self.cnt[e])
        self._mark(tok, reads, writes)
        return tok

    def dma(self, q, sem, out, in_, reads=(), writes=()):
        self.deps(q, reads, writes)
        self.dcnt[id(sem)] = self.dcnt.get(id(sem), 0) + 16
        self.eng[q].dma_start(out=out, in_=in_).then_inc(sem, 16)
        tok = (sem, self.dcnt[id(sem)])
        self._mark(tok, reads, writes)
        return tok


def build_program(n_tiles=8, debug=False):
    nc = bass.Bass("TRN2", target_bir_lowering=False)
    ntok = n_tiles * T
    NG = 4 + 52 + 55
    x_d = nc.dram_tensor("x", [S_LEN, D], F32, kind="ExternalInput").ap()
    mem_d = nc.dram_tensor("mem", [MEM, D], F32, kind="ExternalInput").ap()
    wq32_d = nc.dram_tensor("wq32", [NG, 128, GE], F32, kind="ExternalInput").ap()
    vecs_d = nc.dram_tensor("vecs", [128, NV], F32, kind="ExternalInput").ap()
    cst_d = nc.dram_tensor("cst", [128, NCST], F32, kind="ExternalInput").ap()
    outg_d = nc.dram_tensor("outg", [128, D], F32, kind="ExternalInput").ap()
    out_d = nc.dram_tensor("out", [S_LEN, D], F32, kind="ExternalOutput").ap()
    wq_d = nc.dram_tensor("wq", [NG, 128, GE], BF16, kind="Internal").ap()
    dbg_d = None
    if debug:
        dbg_d = nc.dram_tensor("dbg", [4, T, D], F32, kind="ExternalOutput").ap()

    with contextlib.ExitStack() as es:
        tk = Trk(nc, es)
        es.enter_context(nc.allow_low_precision(reason="bf16 matmul operands, fp32 accumulation"))
        SB = lambda n, s, d: es.enter_context(nc.sbuf_tensor(n, s, d))
        PS = lambda n, s, d: es.enter_context(nc.psum_tensor(n, s, d))
        SEM = lambda n: es.enter_context(nc.semaphore(n))

        vec = SB("vec_sb", [128, NV], F32); R_vec = Res("vec")
        cst = SB("cst_sb", [128, NCST], F32); R_cst = Res("cst")
        identb = SB("identb", [128, 128], BF16)
        causb = SB("causb", [128, 128], BF16)
        wgb = SB("wgb", [128, 128], BF16)
        lbv = SB("lbv", [128, 24], F32)
        negbf = SB("negbf", [4, 2], F32)
        R_c2 = Res("consts2")
        h = SB("h_sb", [128, NTB, D], F32); R_h = [[Res(f"h{tb}_{cb}") for cb in range(4)] for tb in range(NTB)]
        hnT = SB("hnT", [128, 16, T], BF16); R_hnT = [Res(f"hnT{tb}") for tb in range(NTB)]
        ring = SB("ring", [128, NSLOT, GE], BF16); R_slot = [Res(f"slot{i}") for i in range(NSLOT)]
        kmT = SB("kmT", [128, 2, 4, MEM], BF16)
        vm = SB("vm", [128, 2, 2, 512], BF16)
        R_kv = Res("kv")
        CA = SB("CA", [128, 4, 385], F32); CB = SB("CB", [128, 2, 385], F32)
        CAb = SB("CAb", [128, 4, 385], BF16); CBb = SB("CBb", [128, 2, 385], BF16)
        R_C = [Res(f"C{hh}") for hh in range(4)]
        SH = SB("SH", [128, 12, 128], F32); R_S = [Res(f"S{hh}") for hh in range(12)]
        halo_a = SB("halo_a", [128, 12, 3], F32); R_halo_a = Res("halo_a")
        halo_f = SB("halo_f", [128, 2, NJ, 2], F32); R_halo_f = Res("halo_f")
        gcar = SB("gcar", [4, 2], F32); R_gcar = Res("gcar")

        PH = 68 * 1024
        phase = SB("phase", [128, PH], mybir.dt.uint8)

        class Carver:
            def __init__(self):
                self.off = 0

            def mark(self):
                return self.off

            def reset(self, m):
                self.off = m

            def take(self, shape, dt):
                esz = 2 if dt == BF16 else 4
                n = int(np.prod(shape[1:]))
                nbytes = n * esz
                self.off = (self.off + 31) // 32 * 32
                assert self.off + nbytes <= PH, ("phase overflow", self.off + nbytes)
                ap = phase[:, self.off:self.off + nbytes].bitcast(dt)
                self.off += nbytes
                if len(shape) == 3:
                    ap = ap.rearrange("p (a b) -> p a b", b=shape[2])
                elif len(shape) == 4:
                    ap = ap.rearrange("p (a b c) -> p a b c", b=shape[2], c=shape[3])
                return ap[0:shape[0]]

        pbank = [PS(f"pb{i}", [128, 512], F32) for i in range(6)]
        pbb = [PS(f"pbb{i}", [128, 1024], BF16) for i in range(2)]
        R_pb = [Res(f"pb{i}") for i in range(6)]
        R_pbb = [Res(f"pbb{i}") for i in range(2)]
        rot = {"i": 0, "b": 0}

        def next_bank(n=4):
            i = rot["i"] % n
            rot["i"] += 1
            return pbank[i], R_pb[i]

        def next_bbank():
            i = rot["b"] % 2
            rot["b"] += 1
            return pbb[i], R_pbb[i]

        s_cast = [SEM(f"cast{i}") for i in range(3)]
        s_slot = [SEM(f"ld{i}") for i in range(NSLOT)]
        s_misc = SEM("misc"); s_x = SEM("xld"); s_out = SEM("ost")

        tk.dma("sp", s_misc, vec[:], vecs_d[:, :], writes=[R_vec])
        tk.dma("sp", s_misc, cst[:], cst_d[:, :], writes=[R_cst])
        stage_of = lambda g: 0 if g < 4 + 14 else (1 if g < 4 + 52 else 2)
        cast_tok = [None, None, None]
        for g in range(NG):
            st = stage_of(g)
            for half in range(2):
                src = wq32_d[g, :, half * 4096:(half + 1) * 4096].rearrange("p (a b) -> p a b", b=2048)
                dst = wq_d[g, :, half * 4096:(half + 1) * 4096].rearrange("p (a b) -> p a b", b=2048)
                tk.dcnt[id(s_cast[st])] = tk.dcnt.get(id(s_cast[st]), 0) + 16
                nc.gpsimd.dma_start(out=dst, in_=src).then_inc(s_cast[st], 16)
            cast_tok[st] = (s_cast[st], tk.dcnt[id(s_cast[st])])

        seq = list(range(4))
        for _t in range(n_tiles):
            seq += list(range(4, NG))
        state = {"issued": 0, "consumed": 0}

        def issue_next():
            k = state["issued"]
            if k >= len(seq):
                return
            g = seq[k]
            slot = k % NSLOT
            st = stage_of(g)
            tk._wait("sp", (s_cast[st], _cast_total[st]))
            tk.dma("sp", s_slot[slot], ring[:, slot, :], wq_d[g, :, :], writes=[R_slot[slot]])
            state["issued"] += 1

        _cast_total = [0, 0, 0]
        for g in range(NG):
            _cast_total[stage_of(g)] += 32
        for _ in range(NSLOT):
            issue_next()

        def wget():
            k = state["consumed"]
            state["consumed"] += 1
            slot = k % NSLOT
            return ring[:, slot, :], R_slot[slot]

        def wrel():
            issue_next()

        tk.op("dve", lambda e: e.tensor_copy(out=identb[:], in_=cst[:, C_IDENT:C_IDENT + 128]), reads=[R_cst], writes=[R_c2])
        tk.op("dve", lambda e: e.tensor_copy(out=causb[:], in_=cst[:, C_CAUS:C_CAUS + 128]), reads=[R_cst], writes=[R_c2])
        tk.op("dve", lambda e: e.tensor_copy(out=wgb[:], in_=vec[:, V_WGATE:V_WGATE + 128]), reads=[R_vec], writes=[R_c2])
        tk.op("dve", lambda e: e.tensor_tensor(out=lbv[:, 0:12], in0=vec[:, V_LB1:V_LB1 + 12], in1=vec[:, V_LB0:V_LB0 + 12], op=ALU.subtract), reads=[R_vec], writes=[R_c2])
        tk.op("act", lambda e: e.activation(out=lbv[:, 0:12], in_=lbv[:, 0:12], func=AF.Sigmoid), reads=[R_c2], writes=[R_c2])
        tk.op("dve", lambda e: e.tensor_scalar(out=lbv[:, 12:24], in0=lbv[:, 0:12], scalar1=-1.0, scalar2=1.0, op0=ALU.mult, op1=ALU.add), reads=[R_c2], writes=[R_c2])
        tk.op("dve", lambda e: e.tensor_scalar(out=negbf[:, 0:2], in0=vec[0:4, V_GATEB:V_GATEB + 2], scalar1=-1.0, scalar2=0.0, op0=ALU.mult, op1=ALU.add), reads=[R_vec], writes=[R_c2])
        tk.op("pool", lambda e: e.memset(CA[:], 0.0), writes=R_C)
        tk.op("pool", lambda e: e.memset(CB[:], 0.0), writes=R_C)
        tk.op("pool", lambda e: e.memset(CAb[:], 0.0), writes=R_C)
        tk.op("pool", lambda e: e.memset(CBb[:], 0.0), writes=R_C)
        tk.op("pool", lambda e: e.memset(SH[:], 0.0), writes=R_S)
        tk.op("pool", lambda e: e.memset(halo_a[:], 0.0), writes=[R_halo_a])
        tk.op("pool", lambda e: e.memset(halo_f[:], 0.0), writes=[R_halo_f])
        tk.op("pool", lambda e: e.memset(gcar[:], 0.0), writes=[R_gcar])

        identf = cst[:, C_IDENT:C_IDENT + 128]

        def rms_to_T(src, R_src_blk, nblk, gcol, dstT, R_dst_blk, tmpb, R_tmpb, small, R_small):
            for tb in range(nblk):
                tk.op("act", lambda e: e.activation(out=tmpb[:, :], in_=src[:, tb, :], func=AF.Square, accum_out=small[:, 0:1]),
                      reads=R_src_blk[tb], writes=[R_tmpb, R_small])
                tk.op("dve", lambda e: e.tensor_scalar(out=small[:, 1:2], in0=small[:, 0:1], scalar1=1.0 / D, scalar2=EPS, op0=ALU.mult, op1=ALU.add),
                      reads=[R_small], writes=[R_small])
                tk.op("act", lambda e: e.activation(out=small[:, 2:3], in_=small[:, 1:2], func=AF.Sqrt), reads=[R_small], writes=[R_small])
                tk.op("dve", lambda e: e.reciprocal(out=small[:, 3:4], in_=small[:, 2:3]), reads=[R_small], writes=[R_small])
                tk.op("dve", lambda e: e.tensor_scalar(out=tmpb[:, :], in0=src[:, tb, :], scalar1=small[:, 3:4], scalar2=None, op0=ALU.mult),
                      reads=R_src_blk[tb] + [R_small], writes=[R_tmpb])
                for q in range(2):
                    pb, R_p = next_bbank()
                    for c8 in range(8):
                        c = q * 8 + c8
                        tk.op("pe", lambda e: e.transpose(out=pb[:, c8 * 128:(c8 + 1) * 128], in_=tmpb[:, c * 128:(c + 1) * 128], identity=identb[:]),
                              reads=[R_tmpb, R_c2], writes=[R_p])
                    for c8 in range(8):
                        c = q * 8 + c8
                        eng = "dve" if c8 % 2 == 0 else "act"
                        if eng == "dve":
                            tk.op("dve", lambda e: e.tensor_scalar(out=dstT[:, c, tb * 128:(tb + 1) * 128], in0=pb[:, c8 * 128:(c8 + 1) * 128],
                                                                   scalar1=vec[:, gcol + c:gcol + c + 1], scalar2=None, op0=ALU.mult),
                                  reads=[R_p, R_vec], writes=[R_dst_blk[tb]])
                        else:
                            tk.op("act", lambda e: e.activation(out=dstT[:, c, tb * 128:(tb + 1) * 128], in_=pb[:, c8 * 128:(c8 + 1) * 128],
                                                                func=AF.Copy, scale=vec[:, gcol + c:gcol + c + 1]),
                                  reads=[R_p, R_vec], writes=[R_dst_blk[tb]])

        def proj_a(wslot, R_w, ct, rhsT, R_rhs, ncols, pb, R_p, m=128, lhs_override=None):
            w3 = wslot.rearrange("p (k c) -> p k c", c=512)
            for kc in range(16):
                lhsT = w3[:, kc, ct * 128:ct * 128 + m] if lhs_override is None else lhs_override(kc)
                tk.op("pe", lambda e: e.matmul(pb[0:m, 0:ncols], lhsT=lhsT, rhs=rhsT[:, kc, 0:ncols], start=(kc == 0), stop=(kc == 15)),
                      reads=[R_w] + R_rhs, writes=[R_p])

        def proj_b(wslot, R_w, lhsT_T, R_l, tb, pb, R_p, ncols=512):
            w3 = wslot.rearrange("p (k c) -> p k c", c=512)
            for kc in range(16):
                tk.op("pe", lambda e: e.matmul(pb[:, 0:ncols], lhsT=lhsT_T[:, kc, tb * 128:(tb + 1) * 128], rhs=w3[:, kc, 0:ncols],
                                               start=(kc == 0), stop=(kc == 15)),
                      reads=[R_w, R_l[tb]], writes=[R_p])

        def mem_prologue():
            tk.barrier()
            cv = Carver()
            memt = cv.take([128, 2, D], F32); R_memt = [[Res("memt0")], [Res("memt1")]]
            memT = cv.take([128, 16, MEM], BF16); R_memT = [Res("memT0"), Res("memT1")]
            tmpb = cv.take([128, D], BF16); R_tmpb = Res("tmpb")
            small = cv.take([128, 8], F32); R_small = Res("small")
            for mb in range(2):
                tk.dma("sp", s_misc, memt[:, mb, :], mem_d[mb * 128:(mb + 1) * 128, :], writes=R_memt[mb])
            for l in range(2):
                rms_to_T(memt, R_memt, 2, V_GMEM + 16 * l, memT, R_memT, tmpb, R_tmpb, small, R_small)
                wk, R_wk = wget()
                for hh in range(4):
                    pb, R_p = next_bank()
                    proj_a(wk, R_wk, hh, memT, R_memT, MEM, pb, R_p)
                    tk.op("act", lambda e: e.activation(out=kmT[:, l, hh, :], in_=pb[:, 0:MEM], func=AF.Copy), reads=[R_p], writes=[R_kv])
                wrel()
                wv, R_wv = wget()
                for mb in range(2):
                    pb, R_p = next_bank()
                    proj_b(wv, R_wv, memT, R_memT, mb, pb, R_p)
                    tk.op("act", lambda e: e.activation(out=vm[:, l, mb, :], in_=pb[:, :], func=AF.Copy), reads=[R_p], writes=[R_kv])
                wrel()

        mem_prologue()

        def xattn_block(l, b, xqT, R_xq, ymT, R_ymT, P_f, R_Pf, P_b, R_Pb, PT_b, R_PTb, sm, R_sm):
            blk = slice(b * 128, (b + 1) * 128)
            scale = 128.0 ** -0.5
            pbs = [next_bank(), next_bank()]
            for hh in range(4):
                pb, R_p = pbs[hh // 2]
                o = (hh % 2) * 256
                tk.op("pe", lambda e: e.matmul(pb[:, o:o + 256], lhsT=xqT[:, hh, blk], rhs=kmT[:, l, hh, :], start=True, stop=True),
                      reads=[R_xq, R_kv], writes=[R_p])
            for hh in range(4):
                pb, R_p = pbs[hh // 2]
                o = (hh % 2) * 256
                tk.op("dve", lambda e: e.reduce_max(out=sm[:, hh:hh + 1], in_=pb[:, o:o + 256], axis=AX.X), reads=[R_p], writes=[R_sm])
            tk.op("dve", lambda e: e.tensor_scalar(out=sm[:, 4:8], in0=sm[:, 0:4], scalar1=-scale, scalar2=0.0, op0=ALU.mult, op1=ALU.add),
                  reads=[R_sm], writes=[R_sm])
            for hh in range(4):
                pb, R_p = pbs[hh // 2]
                o = (hh % 2) * 256
                tk.op("act", lambda e: e.activation(out=P_f[:, hh, :], in_=pb[:, o:o + 256], func=AF.Exp, bias=sm[:, 4 + hh:5 + hh], scale=scale,
                                                    accum_out=sm[:, 8 + hh:9 + hh]), reads=[R_p, R_sm], writes=[R_Pf, R_sm])
            tk.op("dve", lambda e: e.reciprocal(out=sm[:, 12:16], in_=sm[:, 8:12]), reads=[R_sm], writes=[R_sm])
            for hh in range(4):
                tk.op("dve", lambda e: e.tensor_scalar(out=P_b[:, hh, :], in0=P_f[:, hh, :], scalar1=sm[:, 12 + hh:13 + hh], scalar2=None, op0=ALU.mult),
                      reads=[R_Pf, R_sm], writes=[R_Pb])
            pt, R_pt = next_bbank()
            for hh in range(4):
                for mb in range(2):
                    i8 = hh * 2 + mb
                    tk.op("pe", lambda e: e.transpose(out=pt[:, i8 * 128:(i8 + 1) * 128], in_=P_b[:, hh, mb * 128:(mb + 1) * 128], identity=identb[:]),
                          reads=[R_Pb, R_c2], writes=[R_pt])
            tk.op("act", lambda e: e.activation(out=PT_b[:, :], in_=pt[:, :], func=AF.Copy), reads=[R_pt], writes=[R_PTb])
            po, R_po = next_bank()
            for hh in range(4):
                for mb in range(2):
                    i8 = hh * 2 + mb
                    tk.op("pe", lambda e: e.matmul(po[:, hh * 128:(hh + 1) * 128], lhsT=vm[:, l, mb, hh * 128:(hh + 1) * 128],
                                                   rhs=PT_b[:, i8 * 128:(i8 + 1) * 128], start=(mb == 0), stop=(mb == 1)),
                          reads=[R_kv, R_PTb], writes=[R_po])
            tk.op("dve", lambda e: e.tensor_copy(out=ymT[:, 12:16, blk], in_=po[:, :].rearrange("p (a b) -> p a b", b=128)),
                  reads=[R_po], writes=[R_ymT[b]])

        def out_proj(ymT, R_ymT):
            for cb in range(4):
                w, R_w = wget()
                for tb in range(NTB):
                    pb, R_p = next_bank()
                    proj_b(w, R_w, ymT, R_ymT, tb, pb, R_p)
                    tk.op("dve", lambda e: e.tensor_tensor(out=h[:, tb, cb * 512:(cb + 1) * 512], in0=pb[:, :], in1=h[:, tb, cb * 512:(cb + 1) * 512], op=ALU.add),
                          reads=[R_p, R_h[tb][cb]], writes=[R_h[tb][cb]])
                wrel()

        def ffn(l):
            tk.barrier()
            cv = Carver()
            actT = cv.take([128, NJ, T], BF16); R_act = [Res(f"act{tb}") for tb in range(NTB)]
            tmpb = cv.take([128, D], BF16); R_tmpb = Res("tmpb")
            small = cv.take([128, 8], F32); R_small = Res("small")
            stg = [cv.take([128, T + 2], F32) for _ in range(2)]; R_stg = [Res("stg0"), Res("stg1")]
            acc = [cv.take([128, T], F32) for _ in range(2)]; R_acc = [Res("acc0"), Res("acc1")]
            R_hall = [[r for row in R_h[tb] for r in [row]] for tb in range(NTB)]
            rms_to_T(h, R_hall, NTB, V_GFFN + 16 * l, hnT, R_hnT, tmpb, R_tmpb, small, R_small)
            for gi in range(22):
                w, R_w = wget()
                for jj in range(2):
                    j = 2 * gi + jj
                    pu, R_pu = next_bank()
                    pg, R_pg = next_bank()
                    proj_a(w, R_w, 2 * jj + 1, hnT, R_hnT, T, pg, R_pg)
                    proj_a(w, R_w, 2 * jj, hnT, R_hnT, T, pu, R_pu)
                    s_ = stg[j % 2]; R_s = R_stg[j % 2]; a_ = acc[j % 2]; R_a = R_acc[j % 2]
                    tk.op("pool", lambda e: e.tensor_copy(out=s_[:, 0:2], in_=halo_f[:, l, j, :]), reads=[R_halo_f], writes=[R_s])
                    tk.op("act", lambda e: e.activation(out=s_[:, 2:T + 2], in_=pg[:, :], func=AF.Copy), reads=[R_pg], writes=[R_s])
                    tk.op("pool", lambda e: e.tensor_copy(out=halo_f[:, l, j, :], in_=s_[:, T:T + 2]), reads=[R_s], writes=[R_halo_f])
                    wc = lambda tap: vec[:, V_FCW + (l * 3 + tap) * 44 + j:V_FCW + (l * 3 + tap) * 44 + j + 1]
                    bc = vec[:, V_FCB + l * 44 + j:V_FCB + l * 44 + j + 1]
                    tk.op("dve", lambda e: e.tensor_scalar(out=a_[:, :], in0=s_[:, 2:T + 2], scalar1=wc(2), scalar2=bc, op0=ALU.mult, op1=ALU.add),
                          reads=[R_s, R_vec], writes=[R_a])
                    tk.op("dve", lambda e: e.scalar_tensor_tensor(out=a_[:, :], in0=s_[:, 1:T + 1], scalar=wc(1), in1=a_[:, :], op0=ALU.mult, op1=ALU.add),
                          reads=[R_s, R_vec, R_a], writes=[R_a])
                    tk.op("dve", lambda e: e.scalar_tensor_tensor(out=a_[:, :], in0=s_[:, 0:T], scalar=wc(0), in1=a_[:, :], op0=ALU.mult, op1=ALU.add),
                          reads=[R_s, R_vec, R_a], writes=[R_a])
                    tk.op("act", lambda e: e.activation(out=a_[:, :], in_=a_[:, :], func=AF.Silu), reads=[R_a], writes=[R_a])
                    tk.op("dve", lambda e: e.tensor_tensor(out=actT[:, j, :], in0=pu[:, :], in1=a_[:, :], op=ALU.mult),
                          reads=[R_pu, R_a], writes=R_act)
                wrel()
            for cb in range(4):
                banks = [(pbank[i], R_pb[i]) for i in range(4)]
                for jp in range(4):
                    w, R_w = wget()
                    w3 = w[:, 0:11 * 512].rearrange("p (k c) -> p k c", c=512)
                    for tb in range(NTB):
                        pb, R_p = banks[tb]
                        for jj in range(11):
                            j = jp * 11 + jj
                            tk.op("pe", lambda e: e.matmul(pb[:, :], lhsT=actT[:, j, tb * 128:(tb + 1) * 128], rhs=w3[:, jj, :],
                                                           start=(j == 0), stop=(j == NJ - 1)),
                                  reads=[R_w, R_act[tb]], writes=[R_p])
                    wrel()
                for tb in range(NTB):
                    pb, R_p = banks[tb]
                    tk.op("dve", lambda e: e.tensor_tensor(out=h[:, tb, cb * 512:(cb + 1) * 512], in0=pb[:, :], in1=h[:, tb, cb * 512:(cb + 1) * 512], op=ALU.add),
                          reads=[R_p, R_h[tb][cb]], writes=[R_h[tb][cb]])

        def mlstm_layer():
            l = 0
            tk.barrier()
            cv = Carver()
            QK = cv.take([128, 12, T], BF16); R_QK = [Res(f"qk{i}") for i in range(12)]
            vaug = cv.take([128, NTB, 4, 386], BF16); R_v = [Res(f"v{tb}") for tb in range(NTB)]
            og = cv.take([128, NTB, 1536], BF16); R_og = [Res(f"og{tb}") for tb in range(NTB)]
            xqT = cv.take([128, 4, T], BF16); R_xq = Res("xq")
            gt = cv.take([4, 6, T], F32); R_gt = Res("gt")
            m8 = cv.take([4, 16], F32)
            mk = cv.mark()
            tmpb = cv.take([128, D], BF16); R_tmpb = Res("tmpb")
            small = cv.take([128, 8], F32); R_small = Res("small")
            stg = cv.take([128, T + 3], F32); R_stg = Res("stg")
            acc = cv.take([128, T], F32); R_acc = Res("acc")
            ymT = hnT; R_ymT = R_hnT

            R_hall = [R_h[tb] for tb in range(NTB)]
            rms_to_T(h, R_hall, NTB, V_GMIX + 16 * l, hnT, R_hnT, tmpb, R_tmpb, small, R_small)

            wg3 = wgb[:, :].rearrange("p (k c) -> p k c", c=8)
            pgi, R_pgi = next_bank()
            pgf, R_pgf = next_bank()
            for kc in range(16):
                tk.op("pe", lambda e: e.matmul(pgi[0:4, :], lhsT=wg3[:, kc, 0:4], rhs=hnT[:, kc, :], start=(kc == 0), stop=(kc == 15)),
                      reads=[R_c2] + R_hnT, writes=[R_pgi])
            for kc in range(16):
                tk.op("pe", lambda e: e.matmul(pgf[0:4, :], lhsT=wg3[:, kc, 4:8], rhs=hnT[:, kc, :], start=(kc == 0), stop=(kc == 15)),
                      reads=[R_c2] + R_hnT, writes=[R_pgf])
            G = lambda i: gt[:, i, :]
            G3 = lambda i: gt[:, i, :].rearrange("p (b t) -> p b t", t=128)
            bc4 = lambda ap: ap.rearrange("p (b o) -> p b o", o=1).to_broadcast([4, 4, 128])
            RG = dict(reads=[R_gt], writes=[R_gt])
            tk.op("act", lambda e: e.activation(out=G(0), in_=pgf[0:4, :], func=AF.Exp, bias=negbf[:, 1:2], scale=-1.0), reads=[R_pgf, R_c2], writes=[R_gt])
            tk.op("dve", lambda e: e.tensor_scalar(out=G(0), in0=G(0), scalar1=1.0, scalar2=0.0, op0=ALU.add, op1=ALU.add), **RG)
            tk.op("act", lambda e: e.activation(out=G(0), in_=G(0), func=AF.Ln), **RG)
            tk.op("dve", lambda e: e.tensor_tensor_scan(out=G(1), data0=cst[0:4, C_ONES:C_ONES + T], data1=G(0), initial=gcar[:, 0:1], op0=ALU.mult, op1=ALU.subtract),
                  reads=[R_gt, R_cst, R_gcar], writes=[R_gt])
            tk.op("dve", lambda e: e.scalar_tensor_tensor(out=G(2), in0=pgi[0:4, :], scalar=vec[0:4, V_GATEB:V_GATEB + 1], in1=G(1), op0=ALU.add, op1=ALU.subtract),
                  reads=[R_pgi, R_vec, R_gt], writes=[R_gt])
            tk.op("dve", lambda e: e.tensor_tensor_scan(out=G(3), data0=G(2), data1=G(2), initial=gcar[:, 1:2], op0=ALU.max, op1=ALU.max),
                  reads=[R_gt, R_gcar], writes=[R_gt])
            tk.op("dve", lambda e: e.tensor_copy(out=m8[:, 0:1], in_=gcar[:, 1:2]), reads=[R_gcar, R_gt], writes=[R_gt])
            tk.op("dve", lambda e: e.tensor_copy(out=m8[:, 1:4], in_=G3(3)[:, 0:3, 127]), **RG)
            tk.op("dve", lambda e: e.tensor_copy(out=m8[:, 4:8], in_=G3(3)[:, :, 127]), **RG)
            tk.op("dve", lambda e: e.tensor_tensor(out=m8[:, 8:12], in0=m8[:, 0:4], in1=m8[:, 4:8], op=ALU.subtract), **RG)
            tk.op("dve", lambda e: e.tensor_copy(out=gcar[:, 0:1], in_=gt[:, 1, T - 1:T]), reads=[R_gt], writes=[R_gcar])
            tk.op("dve", lambda e: e.tensor_copy(out=gcar[:, 1:2], in_=gt[:, 3, T - 1:T]), reads=[R_gt], writes=[R_gcar])
            tk.op("dve", lambda e: e.tensor_tensor(out=G3(0), in0=G3(3), in1=bc4(m8[:, 0:4]), op=ALU.subtract), **RG)
            tk.op("act", lambda e: e.activation(out=G(0), in_=G(0), func=AF.Exp, scale=-1.0), **RG)
            tk.op("dve", lambda e: e.tensor_tensor(out=G(1), in0=G(1), in1=G(3), op=ALU.add), **RG)
            tk.op("dve", lambda e: e.tensor_scalar(out=G(1), in0=G(1), scalar1=-1.0, scalar2=-LN_ALPHA, op0=ALU.mult, op1=ALU.add), **RG)
            tk.op("act", lambda e: e.activation(out=G(1), in_=G(1), func=AF.Exp), **RG)
            tk.op("dve", lambda e: e.tensor_tensor(out=G3(4), in0=G3(2), in1=bc4(m8[:, 0:4]), op=ALU.subtract), **RG)
            tk.op("act", lambda e: e.activation(out=G(4), in_=G(4), func=AF.Exp), **RG)
            tk.op("dve", lambda e: e.tensor_tensor(out=G3(5), in0=G3(2), in1=bc4(m8[:, 4:8]), op=ALU.subtract), **RG)
            tk.op("act", lambda e: e.activation(out=G(5), in_=G(5), func=AF.Exp), **RG)
            tk.op("dve", lambda e: e.tensor_copy(out=G3(3), in_=bc4(m8[:, 8:12])), **RG)
            tk.op("act", lambda e: e.activation(out=G(3), in_=G(3), func=AF.Exp), **RG)

            for gi in range(3):
                w, R_w = wget()
                for ct in range(4):
                    ti = gi * 4 + ct
                    pb, R_p = next_bank()
                    proj_a(w, R_w, ct, hnT, R_hnT, T, pb, R_p)
                    tk.op("pool", lambda e: e.tensor_copy(out=stg[:, 0:3], in_=halo_a[:, ti, :]), reads=[R_halo_a], writes=[R_stg])
                    tk.op("act", lambda e: e.activation(out=stg[:, 3:T + 3], in_=pb[:, :], func=AF.Copy), reads=[R_p], writes=[R_stg])
                    tk.op("pool", lambda e: e.tensor_copy(out=halo_a[:, ti, :], in_=stg[:, T:T + 3]), reads=[R_stg], writes=[R_halo_a])
                    wc = lambda tap: vec[:, V_ACW + ti * 4 + tap:V_ACW + ti * 4 + tap + 1]
                    tk.op("dve", lambda e: e.tensor_scalar(out=acc[:, :], in0=stg[:, 3:T + 3], scalar1=wc(3), scalar2=vec[:, V_ACB + ti:V_ACB + ti + 1],
                                                           op0=ALU.mult, op1=ALU.add), reads=[R_stg, R_vec], writes=[R_acc])
                    for tap in range(3):
                        tk.op("dve", lambda e: e.scalar_tensor_tensor(out=acc[:, :], in0=stg[:, tap:tap + T], scalar=wc(tap), in1=acc[:, :], op0=ALU.mult, op1=ALU.add),
                              reads=[R_stg, R_vec, R_acc], writes=[R_acc])
                    tk.op("act", lambda e: e.activation(out=QK[:, ti, :], in_=acc[:, :], func=AF.Silu), reads=[R_acc], writes=[R_QK[ti]])
                wrel()
            for hh in range(4):
                pb, R_p = next_bank()
                tk.op("pe", lambda e: e.matmul(pb[:, :], lhsT=cst[0:4, C_SEL + hh * 128:C_SEL + (hh + 1) * 128], rhs=G(0), start=True, stop=True),
                      reads=[R_cst, R_gt], writes=[R_p])
                tk.op("dve", lambda e: e.tensor_tensor(out=QK[:, hh, :], in0=pb[:, :], in1=QK[:, hh, :], op=ALU.mult), reads=[R_p, R_QK[hh]], writes=[R_QK[hh]])
                j = hh // 2; rows = slice(64 * (hh % 2), 64 * (hh % 2) + 64)
                tk.op("dve", lambda e: e.tensor_tensor(out=QK[rows, 8 + j, :], in0=pb[rows, :], in1=QK[rows, 8 + j, :], op=ALU.mult),
                      reads=[R_p, R_QK[8 + j]], writes=[R_QK[8 + j]])
            w, R_w = wget()
            for ct in range(4):
                pb, R_p = next_bank()
                proj_a(w, R_w, ct, hnT, R_hnT, T, pb, R_p)
                tk.op("act", lambda e: e.activation(out=xqT[:, ct, :], in_=pb[:, :], func=AF.Copy), reads=[R_p], writes=[R_xq])
            wrel()
            for tb in range(NTB):
                tk.op("pool", lambda e: e.memset(vaug[:, tb, :, 384:386], 1.0), writes=[R_v[tb]])
            for gi in range(3):
                w, R_w = wget()
                for tb in range(NTB):
                    pb, R_p = next_bank()
                    proj_b(w, R_w, hnT, R_hnT, tb, pb, R_p)
                    c0 = gi * 512
                    while c0 < (gi + 1) * 512:
                        hh = c0 // 384
                        c1 = min((hh + 1) * 384, (gi + 1) * 512)
                        tk.op("act", lambda e: e.activation(out=vaug[:, tb, hh, c0 - hh * 384:c1 - hh * 384], in_=pb[:, c0 - gi * 512:c1 - gi * 512], func=AF.Copy),
                              reads=[R_p], writes=[R_v[tb]])
                        c0 = c1
                wrel()
            for gi in range(3):
                w, R_w = wget()
                for tb in range(NTB):
                    pb, R_p = next_bank()
                    proj_b(w, R_w, hnT, R_hnT, tb, pb, R_p)
                    tk.op("act", lambda e: e.activation(out=og[:, tb, gi * 512:(gi + 1) * 512], in_=pb[:, :], func=AF.Sigmoid), reads=[R_p], writes=[R_og[tb]])
                wrel()

            tk.barrier()
            cv.reset(mk)
            colsT = cv.take([128, 16], F32); R_cols = Res("cols")
            PT = cv.take([128, 128], BF16); R_PT = Res("PT")
            sq_junk = cv.take([128, 384], BF16); R_junk = Res("junk")
            hs = cv.take([128, 16], F32); R_hs = Res("hs")
            kw = cv.take([128, 192], BF16); R_kw = Res("kw")
            ystage = cv.take([128, 1536], BF16); R_ys = Res("ys")
            P_f = cv.take([128, 4, MEM], F32); R_Pf = Res("Pf")
            P_b = cv.take([128, 4, MEM], BF16); R_Pb = Res("Pb")
            PT_b = cv.take([128, 1024], BF16); R_PTb = Res("PTb")
            sm = cv.take([128, 16], F32); R_sm = Res("sm")
            for b in range(NTB):
                blk = slice(b * 128, (b + 1) * 128)
                pc_, R_pc = pbank[4], R_pb[4]
                for qi, row in enumerate([4, 5, 1, 3]):
                    tk.op("pe", lambda e: e.transpose(out=pc_[:, qi * 4:(qi + 1) * 4], in_=gt[:, row, blk], identity=identf[0:4, 0:4]), reads=[R_gt, R_cst], writes=[R_pc])
                tk.op("dve", lambda e: e.tensor_copy(out=colsT[:, 0:16], in_=pc_[:, 0:16]), reads=[R_pc], writes=[R_cols])
                for hh in range(4):
                    j = hh // 2; hp = hh % 2; rows = slice(64 * hp, 64 * hp + 64)
                    pS, R_pS = pbank[5], R_pb[5]
                    tk.op("pe", lambda e: e.matmul(pS[:, 0:128], lhsT=QK[:, 4 + hh, blk], rhs=QK[:, hh, blk], start=True, stop=False),
                          reads=[R_QK[4 + hh], R_QK[hh]], writes=[R_pS])
                    tk.op("pe", lambda e: e.matmul(pS[:, 0:128], lhsT=QK[rows, 10 + j, blk], rhs=QK[rows, 8 + j, blk], start=False, stop=True),
                          reads=[R_QK[10 + j], R_QK[8 + j]], writes=[R_pS])
                    tk.op("dve", lambda e: e.scalar_tensor_tensor(out=PT[:, :], in0=pS[:, 0:128], scalar=colsT[:, hh:hh + 1], in1=causb[:, :], op0=ALU.mult, op1=ALU.mult),
                          reads=[R_pS, R_cols, R_c2], writes=[R_PT])
                    pN, R_pN = next_bank()
                    tk.op("pe", lambda e: e.matmul(pN[:, 0:385], lhsT=PT[:, :], rhs=vaug[:, b, hh, 0:385], start=True, stop=False),
                          reads=[R_PT, R_v[b]], writes=[R_pN])
                    tk.op("pe", lambda e: e.matmul(pN[:, 0:385], lhsT=QK[:, hh, blk], rhs=CAb[:, hh, :], start=False, stop=False),
                          reads=[R_QK[hh], R_C[hh]], writes=[R_pN])
                    tk.op("pe", lambda e: e.matmul(pN[:, 0:385], lhsT=QK[rows, 8 + j, blk], rhs=CBb[rows, j, :], start=False, stop=True),
                          reads=[R_QK[8 + j], R_C[hh]], writes=[R_pN])
                    H = lambda i: hs[:, i:i + 1]
                    RH = dict(reads=[R_hs], writes=[R_hs])
                    tk.op("dve", lambda e: e.tensor_scalar(out=H(0), in0=pN[:, 384:385], scalar1=-1.0, scalar2=0.0, op0=ALU.mult, op1=ALU.add), reads=[R_pN], writes=[R_hs])
                    tk.op("dve", lambda e: e.tensor_tensor(out=H(0), in0=pN[:, 384:385], in1=H(0), op=ALU.max), reads=[R_pN, R_hs], writes=[R_hs])
                    tk.op("dve", lambda e: e.tensor_tensor(out=H(0), in0=H(0), in1=colsT[:, 8 + hh:9 + hh], op=ALU.max), reads=[R_hs, R_cols], writes=[R_hs])
                    tk.op("dve", lambda e: e.reciprocal(out=H(1), in_=H(0)), **RH)
                    tk.op("act", lambda e: e.activation(out=sq_junk[:, :], in_=pN[:, 0:384], func=AF.Square, accum_out=H(2)), reads=[R_pN], writes=[R_junk, R_hs])
                    tk.op("dve", lambda e: e.tensor_tensor(out=H(3), in0=H(2), in1=H(1), op=ALU.mult), **RH)
                    tk.op("dve", lambda e: e.tensor_tensor(out=H(3), in0=H(3), in1=H(1), op=ALU.mult), **RH)
                    tk.op("dve", lambda e: e.tensor_scalar(out=H(3), in0=H(3), scalar1=1.0 / 384.0, scalar2=EPS, op0=ALU.mult, op1=ALU.add), **RH)
                    tk.op("act", lambda e: e.activation(out=H(4), in_=H(3), func=AF.Sqrt), **RH)
                    tk.op("dve", lambda e: e.reciprocal(out=H(5), in_=H(4)), **RH)
                    tk.op("dve", lambda e: e.tensor_tensor(out=H(6), in0=H(5), in1=H(1), op=ALU.mult), **RH)
                    tk.op("dve", lambda e: e.scalar_tensor_tensor(out=ystage[:, hh * 384:(hh + 1) * 384], in0=pN[:, 0:384], scalar=H(6), in1=og[:, b, hh * 384:(hh + 1) * 384],
                                                                  op0=ALU.mult, op1=ALU.mult), reads=[R_pN, R_hs, R_og[b]], writes=[R_ys])
                    pk, R_pk = next_bbank()
                    tk.op("pe", lambda e: e.transpose(out=pk[:, 0:128], in_=QK[:, 4 + hh, blk], identity=identb[:]), reads=[R_QK[4 + hh], R_c2], writes=[R_pk])
                    tk.op("pe", lambda e: e.transpose(out=pk[:, 128:256], in_=QK[:, 10 + j, blk], identity=identb[:]), reads=[R_QK[10 + j], R_c2], writes=[R_pk])
                    tk.op("dve", lambda e: e.tensor_scalar(out=kw[:, 0:128], in0=pk[:, 0:128], scalar1=colsT[:, 4 + hh:5 + hh], scalar2=None, op0=ALU.mult),
                          reads=[R_pk, R_cols], writes=[R_kw])
                    tk.op("dve", lambda e: e.tensor_scalar(out=kw[:, 128:192], in0=pk[:, 128 + 64 * hp:192 + 64 * hp], scalar1=colsT[:, 4 + hh:5 + hh], scalar2=None, op0=ALU.mult),
                          reads=[R_pk, R_cols], writes=[R_kw])
                    pA, R_pA = next_bank()
                    tk.op("pe", lambda e: e.matmul(pA[:, 0:385], lhsT=kw[:, 0:128], rhs=vaug[:, b, hh, 0:385], start=True, stop=True), reads=[R_kw, R_v[b]], writes=[R_pA])
                    pBk, R_pBk = next_bank()
                    tk.op("pe", lambda e: e.matmul(pBk[rows, 0:385], lhsT=kw[:, 128:192], rhs=vaug[:, b, hh, 0:385], start=True, stop=True), reads=[R_kw, R_v[b]], writes=[R_pBk])
                    tk.op("dve", lambda e: e.scalar_tensor_tensor(out=CA[:, hh, :], in0=CA[:, hh, :], scalar=colsT[:, 12 + hh:13 + hh], in1=pA[:, 0:385], op0=ALU.mult, op1=ALU.add),
                          reads=[R_pA, R_cols, R_C[hh]], writes=[R_C[hh]])
                    tk.op("dve", lambda e: e.scalar_tensor_tensor(out=CB[rows, j, :], in0=CB[rows, j, :], scalar=colsT[rows, 12 + hh:13 + hh], in1=pBk[rows, 0:385], op0=ALU.mult, op1=ALU.add),
                          reads=[R_pBk, R_cols, R_C[hh]], writes=[R_C[hh]])
                    tk.op("act", lambda e: e.activation(out=CAb[:, hh, :], in_=CA[:, hh, :], func=AF.Copy), reads=[R_C[hh]], writes=[R_C[hh]])
                    tk.op("act", lambda e: e.activation(out=CBb[rows, j, :], in_=CB[rows, j, :], func=AF.Copy), reads=[R_C[hh]], writes=[R_C[hh]])
                for q in range(2):
                    pt, R_pt = next_bbank()
                    for c6 in range(6):
                        c = q * 6 + c6
                        tk.op("pe", lambda e: e.transpose(out=pt[:, c6 * 128:(c6 + 1) * 128], in_=ystage[:, c * 128:(c + 1) * 128], identity=identb[:]),
                              reads=[R_ys, R_c2], writes=[R_pt])
                    for c6 in range(6):
                        c = q * 6 + c6
                        tk.op("dve", lambda e: e.tensor_scalar(out=ymT[:, c, blk], in0=pt[:, c6 * 128:(c6 + 1) * 128], scalar1=vec[:, V_AHG + c:V_AHG + c + 1], scalar2=None, op0=ALU.mult),
                              reads=[R_pt, R_vec], writes=[R_ymT[b]])
                xattn_block(l, b, xqT, R_xq, ymT, R_ymT, P_f, R_Pf, P_b, R_Pb, PT_b, R_PTb, sm, R_sm)
            out_proj(ymT, R_ymT)

        def hgrn_layer():
            l = 1
            tk.barrier()
            cv = Carver()
            QP = cv.take([128, 12, T], BF16); R_QP = [Res(f"qp{i}") for i in range(12)]
            KP = cv.take([128, 12, T], BF16); R_KP = [Res(f"kp{i}") for i in range(12)]
            iv = cv.take([128, NTB, 1536], BF16); R_iv = [Res(f"iv{tb}") for tb in range(NTB)]
            sgv = cv.take([128, NTB, 1536], BF16); R_sg = [Res(f"sg{tb}") for tb in range(NTB)]
            xqT = cv.take([128, 4, T], BF16); R_xq = Res("xq")
            ecols = cv.take([128, 12, 12], F32); R_ec = [Res(f"ec{i}") for i in range(12)]
            mk = cv.mark()
            tmpb = cv.take([128, D], BF16); R_tmpb = Res("tmpb")
            small = cv.take([128, 8], F32); R_small = Res("small")
            t1 = cv.take([128, T], F32); t2 = cv.take([128, T], F32); t3 = cv.take([128, T], F32); t4 = cv.take([128, T], F32)
            R_t = [Res(f"t{i}") for i in range(5)]
            ymT = hnT; R_ymT = R_hnT

            R_hall = [R_h[tb] for tb in range(NTB)]
            rms_to_T(h, R_hall, NTB, V_GMIX + 16 * l, hnT, R_hnT, tmpb, R_tmpb, small, R_small)
            v3 = lambda ap: ap.rearrange("p (b t) -> p b t", t=128)
            for hg in range(3):
                wq_, R_wq = wget()
                wf_, R_wf = wget()
                for ct in range(4):
                    hh = hg * 4 + ct
                    pq, R_pq = next_bank()
                    pf, R_pf = next_bank()
                    proj_a(wf_, R_wf, ct, hnT, R_hnT, T, pf, R_pf)
                    proj_a(wq_, R_wq, ct, hnT, R_hnT, T, pq, R_pq)
                    lbc = lbv[:, hh:hh + 1]; omlbc = lbv[:, 12 + hh:13 + hh]
                    tk.op("act", lambda e: e.activation(out=t1[:, :], in_=pf[:, :], func=AF.Sigmoid), reads=[R_pf], writes=[R_t[1]])
                    tk.op("dve", lambda e: e.tensor_scalar(out=t1[:, :], in0=t1[:, :], scalar1=omlbc, scalar2=lbc, op0=ALU.mult, op1=ALU.add), reads=[R_t[1], R_c2], writes=[R_t[1]])
                    tk.op("act", lambda e: e.activation(out=t2[:, :], in_=t1[:, :], func=AF.Ln), reads=[R_t[1]], writes=[R_t[2]])
                    tk.op("dve", lambda e: e.tensor_tensor_scan(out=t3[:, :], data0=cst[:, C_RST:C_RST + T], data1=t2[:, :], initial=0.0, op0=ALU.mult, op1=ALU.add),
                          reads=[R_t[2], R_cst], writes=[R_t[3]])
                    tk.op("pool", lambda e: e.tensor_scalar(out=t1[:, :], in0=t1[:, :], scalar1=-1.0, scalar2=1.0, op0=ALU.mult, op1=ALU.add), reads=[R_t[1], R_t[2]], writes=[R_t[1]])
                    ec = ecols[:, hh, :]
                    tk.op("dve", lambda e: e.tensor_copy(out=ec[:, 0:4], in_=v3(t3)[:, :, 63]), reads=[R_t[3]], writes=[R_ec[hh]])
                    tk.op("dve", lambda e: e.tensor_copy(out=ec[:, 4:8], in_=v3(t3)[:, :, 127]), reads=[R_t[3]], writes=[R_ec[hh]])
                    tk.op("dve", lambda e: e.tensor_tensor(out=ec[:, 8:12], in0=ec[:, 4:8], in1=ec[:, 0:4], op=ALU.subtract), reads=[R_ec[hh]], writes=[R_ec[hh]])
                    tk.op("dve", lambda e: e.tensor_tensor(out=v3(t2), in0=v3(t3), in1=v3(t3)[:, :, 63:64].to_broadcast([128, 4, 128]), op=ALU.subtract),
                          reads=[R_t[3], R_t[2]], writes=[R_t[2]])
                    tk.op("act", lambda e: e.activation(out=ec[:, :], in_=ec[:, :], func=AF.Exp), reads=[R_ec[hh]], writes=[R_ec[hh]])
                    tk.op("act", lambda e: e.activation(out=t4[:, :], in_=t2[:, :], func=AF.Exp), reads=[R_t[2]], writes=[R_t[4]])
                    tk.op("act", lambda e: e.activation(out=t3[:, :], in_=t2[:, :], func=AF.Exp, scale=-1.0), reads=[R_t[2], R_t[3]], writes=[R_t[3]])
                    tk.op("act", lambda e: e.activation(out=t2[:, :], in_=pq[:, :], func=AF.Silu), reads=[R_pq, R_t[2]], writes=[R_t[2]])
                    tk.op("dve", lambda e: e.tensor_tensor(out=QP[:, hh, :], in0=t2[:, :], in1=t4[:, :], op=ALU.mult), reads=[R_t[2], R_t[4]], writes=[R_QP[hh]])
                    tk.op("pool", lambda e: e.tensor_tensor(out=KP[:, hh, :], in0=t1[:, :], in1=t3[:, :], op=ALU.mult), reads=[R_t[1], R_t[3]], writes=[R_KP[hh]])
                wrel(); wrel()
            for gi in range(3):
                w, R_w = wget()
                for tb in range(NTB):
                    pb, R_p = next_bank()
                    proj_b(w, R_w, hnT, R_hnT, tb, pb, R_p)
                    tk.op("act", lambda e: e.activation(out=iv[:, tb, gi * 512:(gi + 1) * 512], in_=pb[:, :], func=AF.Copy), reads=[R_p], writes=[R_iv[tb]])
                wrel()
            for gi in range(3):
                w, R_w = wget()
                for tb in range(NTB):
                    pb, R_p = next_bank()
                    proj_b(w, R_w, hnT, R_hnT, tb, pb, R_p)
                    tk.op("act", lambda e: e.activation(out=sgv[:, tb, gi * 512:(gi + 1) * 512], in_=pb[:, :], func=AF.Silu), reads=[R_p], writes=[R_sg[tb]])
                wrel()
            w, R_w = wget()
            for ct in range(4):
                pb, R_p = next_bank()
                proj_a(w, R_w, ct, hnT, R_hnT, T, pb, R_p)
                tk.op("act", lambda e: e.activation(out=xqT[:, ct, :], in_=pb[:, :], func=AF.Copy), reads=[R_p], writes=[R_xq])
            wrel()

            tk.barrier()
            cv.reset(mk)
            attm = cv.take([128, 128], BF16); R_attm = Res("attm")
            Sb = cv.take([128, 128], BF16); R_Sb = Res("Sb")
            kt = cv.take([128, 128], BF16); R_kt = Res("kt")
            sqb = cv.take([128, 512], F32); R_sqb = Res("sqb")
            hs = cv.take([128, 16], F32); R_hs = Res("hs")
            ystage = cv.take([128, 1536], BF16); R_ys = Res("ys")
            P_f = cv.take([128, 4, MEM], F32); R_Pf = Res("Pf")
            P_b = cv.take([128, 4, MEM], BF16); R_Pb = Res("Pb")
            PT_b = cv.take([128, 1024], BF16); R_PTb = Res("PTb")
            sm = cv.take([128, 16], F32); R_sm = Res("sm")
            for b in range(NTB):
                blk = slice(b * 128, (b + 1) * 128)
                for hgp in range(3):
                    pO, R_pO = pbank[4], R_pb[4]
                    for ct in range(4):
                        hh = hgp * 4 + ct
                        ec = ecols[:, hh, :]
                        pA, R_pA = next_bank()
                        tk.op("pe", lambda e: e.matmul(pA[:, 0:128], lhsT=KP[:, hh, blk], rhs=QP[:, hh, blk], start=True, stop=True), reads=[R_KP[hh], R_QP[hh]], writes=[R_pA])
                        tk.op("dve", lambda e: e.tensor_tensor(out=attm[:, :], in0=pA[:, 0:128], in1=causb[:, :], op=ALU.mult), reads=[R_pA, R_c2], writes=[R_attm])
                        tk.op("dve", lambda e: e.tensor_scalar(out=Sb[:, :], in0=SH[:, hh, :], scalar1=ec[:, b:b + 1], scalar2=None, op0=ALU.mult), reads=[R_S[hh], R_ec[hh]], writes=[R_Sb])
                        tk.op("pe", lambda e: e.matmul(pO[:, ct * 128:(ct + 1) * 128], lhsT=attm[:, :], rhs=iv[:, b, hh * 128:(hh + 1) * 128], start=True, stop=False),
                              reads=[R_attm, R_iv[b]], writes=[R_pO])
                        tk.op("pe", lambda e: e.matmul(pO[:, ct * 128:(ct + 1) * 128], lhsT=QP[:, hh, blk], rhs=Sb[:, :], start=False, stop=True),
                              reads=[R_QP[hh], R_Sb], writes=[R_pO])
                        pk, R_pk = next_bbank()
                        tk.op("pe", lambda e: e.transpose(out=pk[:, 0:128], in_=KP[:, hh, blk], identity=identb[:]), reads=[R_KP[hh], R_c2], writes=[R_pk])
                        tk.op("act", lambda e: e.activation(out=kt[:, :], in_=pk[:, 0:128], func=AF.Copy), reads=[R_pk], writes=[R_kt])
                        pD, R_pD = next_bank()
                        tk.op("pe", lambda e: e.matmul(pD[:, 0:128], lhsT=kt[:, :], rhs=iv[:, b, hh * 128:(hh + 1) * 128], start=True, stop=True), reads=[R_kt, R_iv[b]], writes=[R_pD])
                        tk.op("pool", lambda e: e.tensor_scalar(out=SH[:, hh, :], in0=SH[:, hh, :], scalar1=ec[:, 4 + b:5 + b], scalar2=0.0, op0=ALU.mult, op1=ALU.add),
                              reads=[R_S[hh], R_ec[hh], R_Sb], writes=[R_S[hh]])
                        tk.op("dve", lambda e: e.scalar_tensor_tensor(out=SH[:, hh, :], in0=pD[:, 0:128], scalar=ec[:, 8 + b:9 + b], in1=SH[:, hh, :], op0=ALU.mult, op1=ALU.add),
                              reads=[R_pD, R_ec[hh], R_S[hh]], writes=[R_S[hh]])
                    tk.op("act", lambda e: e.activation(out=sqb[:, :], in_=pO[:, :], func=AF.Square), reads=[R_pO], writes=[R_sqb])
                    tk.op("dve", lambda e: e.reduce_sum(out=hs[:, 0:4], in_=sqb[:, :].rearrange("p (a b) -> p a b", b=128), axis=AX.X), reads=[R_sqb], writes=[R_hs])
                    tk.op("dve", lambda e: e.tensor_scalar(out=hs[:, 4:8], in0=hs[:, 0:4], scalar1=1.0 / 128.0, scalar2=EPS, op0=ALU.mult, op1=ALU.add), reads=[R_hs], writes=[R_hs])
                    tk.op("act", lambda e: e.activation(out=hs[:, 8:12], in_=hs[:, 4:8], func=AF.Sqrt), reads=[R_hs], writes=[R_hs])
                    tk.op("dve", lambda e: e.reciprocal(out=hs[:, 12:16], in_=hs[:, 8:12]), reads=[R_hs], writes=[R_hs])
                    tk.op("dve", lambda e: e.tensor_tensor(out=sqb[:, :].rearrange("p (a b) -> p a b", b=128), in0=pO[:, :].rearrange("p (a b) -> p a b", b=128),
                                                           in1=hs[:, 12:16].rearrange("p (a o) -> p a o", o=1).to_broadcast([128, 4, 128]), op=ALU.mult),
                          reads=[R_pO, R_hs, R_sqb], writes=[R_sqb])
                    tk.op("pool", lambda e: e.tensor_tensor(out=ystage[:, hgp * 512:(hgp + 1) * 512], in0=sqb[:, :], in1=sgv[:, b, hgp * 512:(hgp + 1) * 512], op=ALU.mult),
                          reads=[R_sqb, R_sg[b]], writes=[R_ys])
                for q in range(2):
                    pt, R_pt = next_bbank()
                    for c6 in range(6):
                        c = q * 6 + c6
                        tk.op("pe", lambda e: e.transpose(out=pt[:, c6 * 128:(c6 + 1) * 128], in_=ystage[:, c * 128:(c + 1) * 128], identity=identb[:]),
                              reads=[R_ys, R_c2], writes=[R_pt])
                    for c6 in range(6):
                        c = q * 6 + c6
                        tk.op("dve", lambda e: e.tensor_scalar(out=ymT[:, c, blk], in0=pt[:, c6 * 128:(c6 + 1) * 128], scalar1=vec[:, V_BHG + c:V_BHG + c + 1], scalar2=None, op0=ALU.mult),
                              reads=[R_pt, R_vec], writes=[R_ymT[b]])
                xattn_block(l, b, xqT, R_xq, ymT, R_ymT, P_f, R_Pf, P_b, R_Pb, PT_b, R_PTb, sm, R_sm)
            out_proj(ymT, R_ymT)

        def dump(i):
            if debug:
                for tb in range(NTB):
                    tk.dma("sp", s_out, dbg_d[i, tb * 128:(tb + 1) * 128, :], h[:, tb, :], reads=R_h[tb])

        for t in range(n_tiles):
            t0 = t * T
            for tb in range(NTB):
                tk.dma("sp", s_x, h[:, tb, :], x_d[t0 + tb * 128:t0 + (tb + 1) * 128, :], writes=R_h[tb])
            mlstm_layer()
            if t == 0: dump(0)
            ffn(0)
            if t == 0: dump(1)
            hgrn_layer()
            if t == 0: dump(2)
            ffn(1)
            if t == 0: dump(3)
            tk.barrier()
            cv = Carver()
            junk = cv.take([128, D], BF16); R_junk = Res("fjunk")
            small = cv.take([128, 8], F32); R_small = Res("fsmall")
            outg = cv.take([128, D], F32); R_outg = Res("outg")
            for e_ in ("pe", "act", "dve", "pool"):
                if tk.cnt[e_] > 0:
                    tk._wait("sp", (tk.sem[e_], tk.cnt[e_]))
            tk.dma("sp", s_misc, outg[:, :], outg_d[:, :], writes=[R_outg])
            for tb in range(NTB):
                tk.op("act", lambda e: e.activation(out=junk[:, :], in_=h[:, tb, :], func=AF.Square, accum_out=small[:, 0:1]), reads=R_h[tb], writes=[R_junk, R_small])
                tk.op("dve", lambda e: e.tensor_scalar(out=small[:, 1:2], in0=small[:, 0:1], scalar1=1.0 / D, scalar2=EPS, op0=ALU.mult, op1=ALU.add), reads=[R_small], writes=[R_small])
                tk.op("act", lambda e: e.activation(out=small[:, 2:3], in_=small[:, 1:2], func=AF.Sqrt), reads=[R_small], writes=[R_small])
                tk.op("dve", lambda e: e.reciprocal(out=small[:, 3:4], in_=small[:, 2:3]), reads=[R_small], writes=[R_small])
                tk.op("dve", lambda e: e.scalar_tensor_tensor(out=h[:, tb, :], in0=h[:, tb, :], scalar=small[:, 3:4], in1=outg[:, :], op0=ALU.mult, op1=ALU.mult),
                      reads=R_h[tb] + [R_small, R_outg], writes=R_h[tb])
                tk.dma("sp", s_out, out_d[t0 + tb * 128:t0 + (tb + 1) * 128, :], h[:, tb, :], reads=R_h[tb])
        nc.sync.wait_ge(s_out, tk.dcnt[id(s_out)])
        build_program.stats = dict(cnt=dict(tk.cnt), waits=tk.nw)
    return nc


_CACHE = {}


def kernel(**inputs):
    n_cores = 8
    wq32, vecs, cst = _host_prep(inputs)
    x = np.asarray(inputs["x"], dtype=np.float32)
    mem = np.asarray(inputs["mem"], dtype=np.float32)
    outg = np.ascontiguousarray(np.broadcast_to(np.asarray(inputs["norm_out_g"], dtype=np.float32)[None, :], (128, D)))
    nc = build_program(n_tiles=S_LEN // T, debug=False)
    in_maps = []
    for b in range(n_cores):
        in_maps.append({"x": np.ascontiguousarray(x[b]), "mem": np.ascontiguousarray(mem[b]),
                        "wq32": wq32, "vecs": vecs, "cst": cst, "outg": outg})
    res = run_bass_kernel_spmd(nc, in_maps, core_ids=list(range(n_cores)))
    out = np.stack([np.asarray(res.results[b]["out"]) for b in range(n_cores)], axis=0)
    return out.astype(np.float32)
```
